# Optimizing a Trainium2 kernel written in Bass

```python
import math
import jax, jax.numpy as jnp
from jax import lax
import numpy as np

D_MODEL = 1024
BATCH = 4
SEQ = 4096
DEPTH = 2

DSA_HEADS = 8
DSA_HEAD_DIM = 64
IDX_HEADS = 4
IDX_HEAD_DIM = 64
TOPK_MAX = 256
SB_HEADS = 8
SB_HEAD_DIM = 64
Q_BLOCK = 128
ROPE_THETA = 10000.0
LN_EPS = 1e-5
N_EXPERTS = 16
N_GROUPS = 4
EXPERTS_PER_GROUP = N_EXPERTS // N_GROUPS
TOP_K = 2
D_EXPERT = 512
DN_ALPHA = (2 * DEPTH) ** 0.25
DN_BETA = (8 * DEPTH) ** -0.25

DSA_W = DSA_HEADS * DSA_HEAD_DIM
SB_W = SB_HEADS * SB_HEAD_DIM
IN_SPLITS = (DSA_W, DSA_W, DSA_W, IDX_HEADS * IDX_HEAD_DIM, IDX_HEAD_DIM, IDX_HEADS,
             SB_W, SB_W, SB_W, 2 * D_MODEL)
IN_SCALES = (1.0, 1.0, DN_BETA, 1.0, 1.0, 1.0, 1.0, 1.0, DN_BETA, 1.0)
D_IN = sum(IN_SPLITS)
SPLIT_POINTS = tuple(int(v) for v in np.cumsum(IN_SPLITS)[:-1])

kernel_name = 'hybrid_dsa_stickbreaking_grouped_moe_deepnorm'


def layer_norm(x, g, b):
    x32 = x.astype(jnp.float32)
    mu = jnp.mean(x32, axis=-1, keepdims=True)
    var = jnp.mean(jnp.square(x32 - mu), axis=-1, keepdims=True)
    y = (x32 - mu) * lax.rsqrt(var + LN_EPS) * g.astype(jnp.float32) + b.astype(jnp.float32)
    return y.astype(x.dtype)


def rope(x):
    s, d = x.shape[1], x.shape[-1]
    half = d // 2
    inv_freq = ROPE_THETA ** (-jnp.arange(half, dtype=jnp.float32) / half)
    ang = jnp.arange(s, dtype=jnp.float32)[:, None] * inv_freq[None, :]
    cos, sin = jnp.cos(ang)[:, None, :], jnp.sin(ang)[:, None, :]
    x32 = x.astype(jnp.float32)
    x1, x2 = x32[..., :half], x32[..., half:]
    return jnp.concatenate([x1 * cos - x2 * sin, x2 * cos + x1 * sin], axis=-1).astype(x.dtype)


def to_blocks(a, nb):
    return jnp.moveaxis(a.reshape((a.shape[0], nb, Q_BLOCK) + a.shape[2:]), 1, 0)


def dsa_attention(q, k, v, q_idx, k_idx, w_idx):
    b, s = q.shape[0], q.shape[1]
    n_sel = min(TOPK_MAX, s // 4)
    nb = s // Q_BLOCK
    key_pos = jnp.arange(s)
    k_idx32 = k_idx.astype(jnp.float32)
    w_scale = IDX_HEADS ** -0.5 * IDX_HEAD_DIM ** -0.5

    def one_block(args):
        i, qb, qib, wb = args
        q_pos = i * Q_BLOCK + jnp.arange(Q_BLOCK)
        visible = key_pos[None, :] <= q_pos[:, None]
        logits = jnp.einsum('bqhd,bsd->bqhs', qib.astype(jnp.float32), k_idx32)
        index_score = jnp.einsum('bqhs,bqh->bqs', jax.nn.relu(logits),
                                 wb.astype(jnp.float32) * w_scale)
        index_score = jnp.where(visible[None], index_score, -jnp.inf)
        _, sel = lax.top_k(index_score, n_sel)
        valid = sel <= q_pos[None, :, None]
        k_sel = jax.vmap(lambda kk, ii: kk[ii])(k, sel)
        v_sel = jax.vmap(lambda vv, ii: vv[ii])(v, sel)
        scores = jnp.einsum('bqhd,bqkhd->bhqk', qb, k_sel).astype(jnp.float32) / math.sqrt(DSA_HEAD_DIM)
        scores = jnp.where(valid[:, None], scores, -jnp.inf)
        p = jax.nn.softmax(scores, axis=-1).astype(v.dtype)
        return jnp.einsum('bhqk,bqkhd->bqhd', p, v_sel)

    out = lax.map(one_block, (jnp.arange(nb), to_blocks(q, nb), to_blocks(q_idx, nb),
                              to_blocks(w_idx, nb)))
    return jnp.moveaxis(out, 0, 1).reshape(b, s, -1)


def stick_breaking_attention(q, k, v):
    b, s = q.shape[0], q.shape[1]
    nb = s // Q_BLOCK
    key_pos = jnp.arange(s)

    def one_block(args):
        i, qb = args
        q_pos = i * Q_BLOCK + jnp.arange(Q_BLOCK)
        before = key_pos[None, :] < q_pos[:, None]
        z = jnp.einsum('bqhd,bshd->bhqs', qb, k).astype(jnp.float32) / math.sqrt(SB_HEAD_DIM)
        log_beta = jax.nn.log_sigmoid(z)
        log_one_minus = jnp.where(before, jax.nn.log_sigmoid(-z), 0.0)
        later = lax.cumsum(log_one_minus, axis=3, reverse=True) - log_one_minus
        att = jnp.where(before, jnp.exp(log_beta + later), 0.0).astype(v.dtype)
        return jnp.einsum('bhqs,bshd->bqhd', att, v)

    out = lax.map(one_block, (jnp.arange(nb), to_blocks(q, nb)))
    return jnp.moveaxis(out, 0, 1).reshape(b, s, -1)


def token_mixer(x, w_in, b_gate, idx_k_norm_g, idx_k_norm_b, w_branch_a, w_branch_b, w_out):
    b, s, _ = x.shape
    proj = jnp.einsum('bsd,de->bse', x, w_in)
    q_a, k_a, v_a, q_i, k_i, w_i, q_b, k_b, v_b, gates = jnp.split(proj, SPLIT_POINTS, axis=-1)
    q_a = rope(q_a.reshape(b, s, DSA_HEADS, DSA_HEAD_DIM))
    k_a = rope(k_a.reshape(b, s, DSA_HEADS, DSA_HEAD_DIM))
    v_a = v_a.reshape(b, s, DSA_HEADS, DSA_HEAD_DIM)
    q_i = rope(q_i.reshape(b, s, IDX_HEADS, IDX_HEAD_DIM))
    k_i = rope(layer_norm(k_i, idx_k_norm_g, idx_k_norm_b)[:, :, None, :])[:, :, 0, :]
    y_a = dsa_attention(q_a, k_a, v_a, q_i, k_i, w_i)
    y_b = stick_breaking_attention(q_b.reshape(b, s, SB_HEADS, SB_HEAD_DIM),
                                   k_b.reshape(b, s, SB_HEADS, SB_HEAD_DIM),
                                   v_b.reshape(b, s, SB_HEADS, SB_HEAD_DIM))
    g = jax.nn.sigmoid((gates + b_gate).astype(jnp.float32)).astype(x.dtype)
    g_a, g_b = g[..., :D_MODEL], g[..., D_MODEL:]
    merged = g_a * jnp.einsum('bse,ed->bsd', y_a, w_branch_a) + g_b * jnp.einsum('bse,ed->bsd', y_b, w_branch_b)
    return jnp.einsum('bsd,de->bse', merged, w_out)


def grouped_moe(x, w_router, router_bias, exp_w_gate, exp_w_up, exp_w_down):
    b, s, d = x.shape
    xf = x.reshape(b * s, d)
    n = xf.shape[0]
    scores = jax.nn.sigmoid(jnp.einsum('nd,de->ne', xf, w_router).astype(jnp.float32))
    biased = scores + router_bias.astype(jnp.float32)
    group_score = lax.top_k(biased.reshape(n, N_GROUPS, EXPERTS_PER_GROUP), TOP_K)[0].sum(-1)
    g_sel = jnp.argmax(group_score, axis=-1)
    in_group = (jnp.arange(N_EXPERTS) // EXPERTS_PER_GROUP)[None, :] == g_sel[:, None]
    _, idx = lax.top_k(jnp.where(in_group, biased, -jnp.inf), TOP_K)
    w = jnp.take_along_axis(scores, idx, axis=-1)
    w = w / jnp.sum(w, axis=-1, keepdims=True)
    combine = jnp.sum(jax.nn.one_hot(idx, N_EXPERTS, dtype=jnp.float32) * w[..., None], axis=1).astype(x.dtype)
    y = jnp.zeros_like(xf)
    for e in range(N_EXPERTS):
        h = jax.nn.silu(xf @ exp_w_gate[e]) * (xf @ exp_w_up[e])
        y = y + combine[:, e:e + 1] * (h @ exp_w_down[e])
    return y.reshape(b, s, d)


def setup_inputs(seed: int = 0) -> dict:
    key = jax.random.key(seed)
    ks = jax.random.split(key, 17)
    f32 = jnp.float32
    col_scale = jnp.concatenate([jnp.full((n,), sc, f32) for n, sc in zip(IN_SPLITS, IN_SCALES)])
    nrm = lambda k, shp: jax.random.normal(k, shp, f32)
    return {
        'x': nrm(ks[0], (BATCH, SEQ, D_MODEL)),
        'w_in': nrm(ks[1], (DEPTH, D_MODEL, D_IN)) * D_MODEL ** -0.5 * col_scale,
        'b_gate': 0.1 * nrm(ks[2], (DEPTH, 2 * D_MODEL)),
        'idx_k_norm_g': 1.0 + 0.05 * nrm(ks[3], (DEPTH, IDX_HEAD_DIM)),
        'idx_k_norm_b': 0.02 * nrm(ks[4], (DEPTH, IDX_HEAD_DIM)),
        'w_branch_a': nrm(ks[5], (DEPTH, DSA_W, D_MODEL)) * DSA_W ** -0.5 * DN_BETA,
        'w_branch_b': nrm(ks[6], (DEPTH, SB_W, D_MODEL)) * SB_W ** -0.5 * DN_BETA,
        'w_out': nrm(ks[7], (DEPTH, D_MODEL, D_MODEL)) * D_MODEL ** -0.5 * DN_BETA,
        'ln1_g': 1.0 + 0.05 * nrm(ks[8], (DEPTH, D_MODEL)),
        'ln1_b': 0.02 * nrm(ks[9], (DEPTH, D_MODEL)),
        'w_router': nrm(ks[10], (D_MODEL, N_EXPERTS)) * D_MODEL ** -0.5,
        'router_bias': 0.01 * nrm(ks[11], (N_EXPERTS,)),
        'exp_w_gate': nrm(ks[12], (DEPTH, N_EXPERTS, D_MODEL, D_EXPERT)) * D_MODEL ** -0.5 * DN_BETA,
        'exp_w_up': nrm(ks[13], (DEPTH, N_EXPERTS, D_MODEL, D_EXPERT)) * D_MODEL ** -0.5 * DN_BETA,
        'exp_w_down': nrm(ks[14], (DEPTH, N_EXPERTS, D_EXPERT, D_MODEL)) * D_EXPERT ** -0.5 * DN_BETA,
        'ln2_g': 1.0 + 0.05 * nrm(ks[15], (DEPTH, D_MODEL)),
        'ln2_b': 0.02 * nrm(ks[16], (DEPTH, D_MODEL)),
    }


def reference(x, w_in, b_gate, idx_k_norm_g, idx_k_norm_b, w_branch_a, w_branch_b, w_out,
              ln1_g, ln1_b, w_router, router_bias, exp_w_gate, exp_w_up, exp_w_down,
              ln2_g, ln2_b):
    for l in range(DEPTH):
        mix = token_mixer(x, w_in[l], b_gate[l], idx_k_norm_g[l], idx_k_norm_b[l],
                          w_branch_a[l], w_branch_b[l], w_out[l])
        x = layer_norm(DN_ALPHA * x + mix, ln1_g[l], ln1_b[l])
        ffn = grouped_moe(x, w_router, router_bias, exp_w_gate[l], exp_w_up[l], exp_w_down[l])
        x = layer_norm(DN_ALPHA * x + ffn, ln2_g[l], ln2_b[l])
    return x
```

```python
import math
import os
from contextlib import ExitStack

import numpy as np
import concourse.bass as bass
import concourse.mybir as mybir
from concourse.bass_utils import run_bass_kernel_spmd

F32 = mybir.dt.float32
BF16 = mybir.dt.bfloat16
AF = mybir.ActivationFunctionType
ALU = mybir.AluOpType
AX = mybir.AxisListType

D = 1024
H = 8
DH = 64
NE = 16
FE = 512
LN_EPS = 1e-5
DEPTH = 2
DN_ALPHA = (2 * DEPTH) ** 0.25
NEG = -1.0e30
BIG = 1.0e30

ENGS = ("pe", "dve", "act", "pool", "sp")
NLANES = 8

C_QA, C_QAP, C_KA, C_KAP, C_QI, C_QIP, C_KI, C_QB, C_KB, C_G = 0, 4, 8, 12, 16, 18, 20, 21, 25, 29
NFC = 45


class Prog:
    def __init__(self, nc):
        self.nc = nc
        self.ops = {e: [] for e in ENGS}
        self.cnt = {}
        self.seen = {e: {} for e in ENGS}
        self.lastw = {}
        self.readers = {}
        self.lane_rr = {"sp": 0, "pool": 0, "act": 0}
        self.semkeys = list(ENGS)
        for q in ("sp", "pool"):
            for l in range(NLANES):
                self.semkeys.append(("dma", q, l))
        for k in self.semkeys:
            self.cnt[k] = 0
        self.n_ops = 0
        self.fill_regs = {}

    def fill(self, eng, val):
        if val not in self.fill_regs:
            self.fill_regs[val] = eng.to_reg(val)
        return self.fill_regs[val]

    def _deps(self, reads, writes):
        need = {}

        def add(sk, c):
            if c > need.get(sk, 0):
                need[sk] = c
        for k in reads:
            if k in self.lastw:
                add(*self.lastw[k])
        for k in writes:
            if k in self.lastw:
                add(*self.lastw[k])
            for sk, c in self.readers.get(k, {}).items():
                add(sk, c)
        return need

    def _commit(self, semkey, count, reads, writes):
        for k in reads:
            d = self.readers.setdefault(k, {})
            if count > d.get(semkey, 0):
                d[semkey] = count
        for k in writes:
            self.lastw[k] = (semkey, count)
            self.readers[k] = {}

    def op(self, eng, fn, reads=(), writes=()):
        need = self._deps(reads, writes)
        waits = []
        for sk, c in need.items():
            if self.seen[eng].get(sk, 0) >= c:
                continue
            self.seen[eng][sk] = c
            waits.append((sk, c))
        self.cnt[eng] += 1
        self.ops[eng].append((waits, fn, eng, 1))
        self._commit(eng, self.cnt[eng], reads, writes)
        self.n_ops += 1

    def dma(self, q, fn, reads=(), writes=()):
        lane = self.lane_rr[q]
        self.lane_rr[q] = (lane + 1) % NLANES
        sk = ("dma", q, lane)
        need = self._deps(reads, writes)
        if self.cnt[sk] > 0:
            need[sk] = max(need.get(sk, 0), self.cnt[sk])
        waits = []
        for k2, c in need.items():
            if self.seen[q].get(k2, 0) >= c:
                continue
            self.seen[q][k2] = c
            waits.append((k2, c))
        self.cnt[sk] += 1
        self.ops[q].append((waits, fn, sk, 16))
        self._commit(sk, self.cnt[sk], reads, writes)
        self.n_ops += 1

    def barrier_all(self):
        for e in ENGS:
            waits = []
            for sk in self.semkeys:
                c = self.cnt[sk]
                if c > 0 and self.seen[e].get(sk, 0) < c:
                    self.seen[e][sk] = c
                    waits.append((sk, c))
            if waits:
                self.ops[e].append((waits, None, None, 0))

    def emit(self, sems):
        nc = self.nc
        engmap = {"pe": "tensor", "dve": "vector", "act": "scalar", "pool": "gpsimd", "sp": "sync"}
        ops = self.ops
        self.ops = {e: [] for e in ENGS}

        def make(e):
            def body(eng):
                for waits, fn, sk, inc in ops[e]:
                    for (wk, c) in waits:
                        eng.wait_ge(sems[wk], c * (16 if isinstance(wk, tuple) else 1))
                    if fn is not None:
                        ins = fn(eng)
                        ins.then_inc(sems[sk], inc)
            return body

        with nc.Block() as block:
            for e in ENGS:
                if ops[e]:
                    getattr(block, engmap[e])(make(e))


class Ring:
    def __init__(self, bufs, name):
        self.bufs = bufs
        self.name = name
        self.i = 0

    def next(self):
        j = self.i % len(self.bufs)
        self.i += 1
        return self.bufs[j], (self.name, j)


def build_program(S, L=2, NSEL=256, stop_after=None, debug=False):
    NB = S // 128
    NT = S // 512
    assert S % 512 == 0
    nc = bass.Bass("TRN2", target_bir_lowering=False)
    din = lambda n, s, dt=F32: nc.dram_tensor(n, s, dt, kind="ExternalInput").ap()
    x_in = din("x", [S, D])
    wf = din("wf", [L, D, NFC * 128])
    wt = din("wt", [L, D, 1028])
    wa_d = din("wa", [L, 512, D])
    wb_d = din("wb", [L, 512, D])
    wo_d = din("wo", [L, D, D])
    bg_d = din("bg", [L, 128, 16])
    kng_d = din("kng", [L, 128])
    knb_d = din("knb", [L, 128])
    ln_d = din("lnp", [L, 4, D])
    wr_d = din("wr", [D, NE])
    rb_d = din("rb", [NE])
    eg_d = din("eg", [L, NE, D, FE])
    eu_d = din("eu", [L, NE, D, FE])
    ed_d = din("ed", [L, NE, FE, D])
    c_mats = din("c_mats", [4, 128, 128])
    c_cos = din("c_cos", [128, S])
    c_sin = din("c_sin", [128, S])
    out_d = nc.dram_tensor("out", [S, D], F32, kind="ExternalOutput").ap()
    sk_ = "ExternalOutput" if debug else "Internal"
    xT_d = nc.dram_tensor("xT_d", [128, 8, S], BF16, kind=sk_).ap()
    xres_d = nc.dram_tensor("xres_d", [S, D], F32, kind=sk_).ap()
    yaT_d = nc.dram_tensor("yaT_d", [128, 4, S], BF16, kind=sk_).ap()
    ybT_d = nc.dram_tensor("ybT_d", [128, 4, S], BF16, kind=sk_).ap()
    mk_d = nc.dram_tensor("mk_d", [S, S], BF16, kind=sk_).ap() if debug else None
    is_d = nc.dram_tensor("is_d", [S, S], F32, kind=sk_).ap() if debug else None

    with ExitStack() as es:
        P = Prog(nc)
        sems = {}
        for i, sk in enumerate(P.semkeys):
            sems[sk] = es.enter_context(nc.semaphore("s%d" % i))
        _uid = [0]

        def uniq(name):
            _uid[0] += 1
            return "sb%d_%s" % (_uid[0], name)
        gsb = lambda name, shape, dt: es.enter_context(nc.sbuf_tensor(uniq(name), shape, dt))
        pb = [es.enter_context(nc.psum_tensor("pb%d" % i, [128, 512], F32)) for i in range(8)]
        pk = [("pb", i) for i in range(8)]
        ring_par = [Ring([(pb[0], pk[0]), (pb[1], pk[1])], "rp0"), Ring([(pb[2], pk[2]), (pb[3], pk[3])], "rp1")]
        ring_gen4 = Ring([(pb[4], pk[4]), (pb[5], pk[5]), (pb[6], pk[6]), (pb[7], pk[7])], "rg4")
        ring_gen2 = Ring([(pb[4], pk[4]), (pb[5], pk[5])], "rg2")
        ring_o = Ring([(pb[6], pk[6]), (pb[7], pk[7])], "ro")
        gen_mode = [4]

        def bank_par(g):
            (t, k), _ = ring_par[g].next()
            return t, k

        def bank_gen():
            (t, k), _ = (ring_gen4 if gen_mode[0] == 4 else ring_gen2).next()
            return t, k

        def bank_o():
            (t, k), _ = ring_o.next()
            return t, k

        identF = gsb("identF", [128, 128], F32)
        identB = gsb("identB", [128, 128], BF16)
        Ubf = gsb("Ubf", [128, 128], BF16)
        onesB = gsb("onesB", [128, 128], BF16)
        onesblk = gsb("onesblk", [128, 128], F32)
        Rperm = gsb("Rperm", [128, 128], F32)
        wr_sb = gsb("wr_sb", [128, 8, NE], F32)
        rb_sb = gsb("rb_sb", [128, NE], F32)
        comb = gsb("comb", [128, NB, NE], F32)
        P.dma("sp", lambda e: e.dma_start(out=identF[:], in_=c_mats[0]), writes=["identF"])
        P.dma("pool", lambda e: e.dma_start(out=identB[:], in_=c_mats[0]), writes=["identB"])
        P.dma("pool", lambda e: e.dma_start(out=Ubf[:], in_=c_mats[1]), writes=["Ubf"])
        P.dma("sp", lambda e: e.dma_start(out=onesblk[:], in_=c_mats[2]), writes=["onesblk"])
        P.dma("sp", lambda e: e.dma_start(out=Rperm[:], in_=c_mats[3]), writes=["Rperm"])
        P.dma("sp", lambda e: e.dma_start(out=wr_sb[:], in_=wr_d.rearrange("(c p) n -> p c n", p=128)), writes=["wr_sb"])
        P.dma("sp", lambda e: e.dma_start(out=rb_sb[:], in_=rb_d.partition_broadcast(128)), writes=["rb_sb"])
        P.op("pool", lambda e: e.memset(onesB[:], 1.0), writes=["onesB"])

        def transposes_f32(src, src_key, dst_bf, dst_key, dst_f32=None, dst_f32_key=None):
            for g in range(2):
                bt, bk = bank_gen()

                def tr(e, g=g, bt=bt):
                    for j in range(4):
                        c = g * 4 + j
                        ins = e.transpose(bt[:, j * 128:(j + 1) * 128], src[:, c * 128:(c + 1) * 128], identF[:])
                    return ins
                P.op("pe", tr, reads=[src_key, "identF"], writes=[bk])
                if dst_f32 is None:
                    P.op("act", lambda e, g=g, bt=bt: e.activation(
                        out=dst_bf[:, g * 4:(g + 1) * 4, :], in_=bt[:].rearrange("p (c t) -> p c t", c=4), func=AF.Copy),
                        reads=[bk], writes=[dst_key])
                else:
                    P.op("dve", lambda e, g=g, bt=bt: e.tensor_copy(
                        dst_f32[:, g * 4:(g + 1) * 4, :], bt[:].rearrange("p (c t) -> p c t", c=4)),
                        reads=[bk], writes=[(dst_f32_key, g)])
                    P.op("act", lambda e, g=g: e.activation(
                        out=dst_bf[:, g * 4:(g + 1) * 4, :], in_=dst_f32[:, g * 4:(g + 1) * 4, :], func=AF.Copy),
                        reads=[(dst_f32_key, g)], writes=[dst_key])

        def proj_fm(bank, bkey, w, wkey, c, xt, xkey, ncols=512):
            def f(e):
                for dc in range(8):
                    ins = e.matmul(bank[:, 0:ncols], w[:, dc, c * 128:(c + 1) * 128], xt[:, dc, 0:ncols],
                                   start=(dc == 0), stop=(dc == 7))
                return ins
            P.op("pe", f, reads=[wkey, xkey], writes=[bkey])

        def layer_norm_block(ph, r, rkey, gb, gbkey, gi, outt, outkey, small):
            st6, mv, rstd = small
            for hf in range(2):
                P.op("dve", lambda e, hf=hf: e.bn_stats(out=st6[:, hf, :], in_=r[:, hf * 512:(hf + 1) * 512]),
                     reads=[rkey], writes=["st6"])
            P.op("dve", lambda e: e.bn_aggr(out=mv[:], in_=st6[:].rearrange("p a b -> p (a b)")),
                 reads=["st6"], writes=["mv"])
            P.op("act", lambda e: e.activation(out=rstd[:], in_=mv[:, 1:2], func=AF.Sqrt, bias=epsT[:], scale=1.0),
                 reads=["mv", "epsT"], writes=["rstd"])
            P.op("dve", lambda e: e.reciprocal(out=rstd[:], in_=rstd[:]), reads=["rstd"], writes=["rstd"])
            P.op("dve", lambda e: e.tensor_scalar(out=outt[:], in0=r[:], scalar1=mv[:, 0:1], scalar2=rstd[:, 0:1],
                                                  op0=ALU.subtract, op1=ALU.mult),
                 reads=[rkey, "mv", "rstd"], writes=[outkey])
            P.op("pool", lambda e: e.tensor_tensor(out=outt[:], in0=outt[:], in1=gb[:, gi, :], op=ALU.mult),
                 reads=[outkey, gbkey], writes=[outkey])
            P.op("pool", lambda e: e.tensor_tensor(out=outt[:], in0=outt[:], in1=gb[:, gi + 1, :], op=ALU.add),
                 reads=[outkey, gbkey], writes=[outkey])

        epsT = gsb("epsT", [128, 1], F32)
        P.op("pool", lambda e: e.memset(epsT[:], LN_EPS), writes=["epsT"])

        with ExitStack() as ph:
            sb = lambda name, shape, dt: ph.enter_context(nc.sbuf_tensor(uniq(name), shape, dt))
            xblk = Ring([sb("p0x%d" % i, [128, D], F32) for i in range(3)], "p0x")
            xtt = Ring([sb("p0t%d" % i, [128, 8, 512], BF16) for i in range(2)], "p0t")
            for tt in range(NT):
                tbuf, tkey = xtt.next()
                for j in range(4):
                    b = tt * 4 + j
                    xb, xk = xblk.next()
                    P.dma("sp", lambda e, xb=xb, b=b: e.dma_start(out=xb[:], in_=x_in[b * 128:(b + 1) * 128, :]), writes=[xk])
                    transposes_f32(xb, xk, tbuf[:, :, j * 128:(j + 1) * 128], tkey)
                P.dma("sp", lambda e, tbuf=tbuf, tt=tt: e.dma_start(out=xT_d[:, :, tt * 512:(tt + 1) * 512], in_=tbuf[:]),
                      reads=[tkey], writes=[("xT_d", tt)])
            P.barrier_all()
            P.emit(sems)

        for l in range(L):
            xsrc = x_in if l == 0 else xres_d
            last = (l == L - 1)
            with ExitStack() as ph:
                sb = lambda name, shape, dt: ph.enter_context(nc.sbuf_tensor(uniq(name), shape, dt))
                KA = sb("KA", [128, 4, S], BF16)
                VA = sb("VA", [128, NB, 8, 65], BF16)
                KI = sb("KI", [128, S], BF16)
                wbig = sb("wbig", [128, 8, 1024], BF16)
                wmid = sb("wmid", [128, 8, 512], BF16)
                wki = sb("wki", [128, 8, 128], BF16)
                wwi = sb("wwi", [128, 8, 4], BF16)
                kng = sb("kng", [128, 1], F32)
                knb = sb("knb", [128, 1], F32)
                xtr = Ring([sb("xtA%d" % i, [128, 8, 512], BF16) for i in range(2)], "xtA")
                csr = Ring([sb("csA%d" % i, [128, 2, 512], F32) for i in range(2)], "csA")
                t1r = Ring([sb("t1A%d" % i, [128, 512], F32) for i in range(2)], "t1A")
                t2r = Ring([sb("t2A%d" % i, [128, 512], F32) for i in range(2)], "t2A")
                QA = sb("QA", [128, 4, 512], BF16)
                QI = sb("QI", [128, 2, 512], BF16)
                wiT = sb("wiT", [128, 4, 4], F32)
                absw = sb("absw", [128, 4, 4], F32)
                sgnw = sb("sgnw", [128, 4, 4], F32)
                IS = sb("IS", [128, S], F32)
                Mk = sb("Mk", [128, S], BF16)
                MT = sb("MT", [128, NB, 128], BF16)
                m8 = sb("m8", [128, 8], F32)
                rr = Ring([sb("rA%d" % i, [128, 512], F32) for i in range(2)], "rA")
                Er = Ring([sb("EA%d" % i, [128, 4, 128], BF16) for i in range(3)], "EA")
                Yr = Ring([sb("YA%d" % i, [128, 512], BF16) for i in range(2)], "YA")
                YTr = Ring([sb("YTA%d" % i, [128, 4, 128], BF16) for i in range(2)], "YTA")
                rden = sb("rdenA", [128, 4], F32)

                def load_w(dst, dkey, c0, nch, l=l):
                    P.dma("pool", lambda e: e.dma_start(
                        out=dst[:, :, 0:nch * 128],
                        in_=wf[l, :, c0 * 128:(c0 + nch) * 128].rearrange("(c p) n -> p c n", p=128)), writes=[dkey])

                load_w(wbig, "wbig", C_KA, 8)
                load_w(wki, "wki", C_KI, 1)
                P.dma("pool", lambda e, l=l: e.dma_start(out=wmid[:], in_=wt[l, :, 0:512].rearrange("(c p) n -> p c n", p=128)),
                      writes=["wmid"])
                P.dma("sp", lambda e, l=l: e.dma_start(out=kng[:], in_=kng_d[l].unsqueeze(1)), writes=["kng"])
                P.dma("sp", lambda e, l=l: e.dma_start(out=knb[:], in_=knb_d[l].unsqueeze(1)), writes=["knb"])
                P.op("pool", lambda e: e.memset(VA[:, :, :, 64:65], 1.0), writes=["VAones"])

                def load_tile(tt):
                    xt, xk = xtr.next()
                    P.dma("sp", lambda e: e.dma_start(out=xt[:], in_=xT_d[:, :, tt * 512:(tt + 1) * 512]),
                          reads=[("xT_d", tt)], writes=[xk])
                    cs, ck = csr.next()
                    P.dma("sp", lambda e: e.dma_start(out=cs[:, 0, :], in_=c_cos[:, tt * 512:(tt + 1) * 512]), writes=[ck])
                    P.dma("sp", lambda e: e.dma_start(out=cs[:, 1, :], in_=c_sin[:, tt * 512:(tt + 1) * 512]), writes=[ck])
                    return xt, xk, cs, ck

                def rope_chunk(w, wkey, c_plain, c_perm, xt, xk, cs, ck, dst, dkey):
                    b1, k1 = bank_gen()
                    proj_fm(b1, k1, w, wkey, c_plain, xt, xk)
                    b2, k2 = bank_gen()
                    proj_fm(b2, k2, w, wkey, c_perm, xt, xk)
                    t1, t1k = t1r.next()
                    t2, t2k = t2r.next()
                    P.op("dve", lambda e: e.tensor_tensor(out=t1[:], in0=b1[:], in1=cs[:, 0, :], op=ALU.mult),
                         reads=[k1, ck], writes=[t1k])
                    P.op("dve", lambda e: e.tensor_tensor(out=t2[:], in0=b2[:], in1=cs[:, 1, :], op=ALU.mult),
                         reads=[k2, ck], writes=[t2k])
                    P.op("pool", lambda e: e.tensor_tensor(out=dst, in0=t1[:], in1=t2[:], op=ALU.add),
                         reads=[t1k, t2k], writes=[dkey])

                for tt in range(NT):
                    xt, xk, cs, ck = load_tile(tt)
                    for c in range(4):
                        rope_chunk(wbig, "wbig", c, 4 + c, xt, xk, cs, ck, KA[:, c, tt * 512:(tt + 1) * 512], ("KA", tt))
                    for j in range(4):
                        b = tt * 4 + j
                        bt, bk = bank_gen()

                        def fv(e, bt=bt, j=j, xt=xt):
                            for dc in range(8):
                                ins = e.matmul(bt[:], xt[:, dc, j * 128:(j + 1) * 128], wmid[:, dc, :],
                                               start=(dc == 0), stop=(dc == 7))
                            return ins
                        P.op("pe", fv, reads=[xk, "wmid"], writes=[bk])
                        P.op("act", lambda e, bt=bt, b=b: e.activation(
                            out=VA[:, b, :, 0:64], in_=bt[:].rearrange("p (h d) -> p h d", h=8), func=AF.Copy),
                            reads=[bk], writes=[("VA", b)])
                    bt, bk = bank_gen()
                    proj_fm(bt, bk, wki, "wki", 0, xt, xk)
                    k0, k0k = t1r.next()
                    P.op("act", lambda e, bt=bt, k0=k0: e.activation(out=k0[:], in_=bt[:], func=AF.Copy), reads=[bk], writes=[k0k])
                    bm, bmk = bank_gen()
                    P.op("pe", lambda e, bm=bm, k0=k0: e.matmul(bm[:], onesblk[:], k0[:], start=True, stop=True),
                         reads=["onesblk", k0k], writes=[bmk])
                    cen, cenk = t2r.next()
                    P.op("dve", lambda e, cen=cen, k0=k0, bm=bm: e.tensor_tensor(out=cen[:], in0=k0[:], in1=bm[:], op=ALU.subtract),
                         reads=[k0k, bmk], writes=[cenk])
                    sq, sqk = t1r.next()
                    P.op("act", lambda e, sq=sq, cen=cen: e.activation(out=sq[:], in_=cen[:], func=AF.Square), reads=[cenk], writes=[sqk])
                    bv, bvk = bank_gen()
                    P.op("pe", lambda e, bv=bv, sq=sq: e.matmul(bv[:], onesblk[:], sq[:], start=True, stop=True),
                         reads=["onesblk", sqk], writes=[bvk])
                    rs, rsk = t1r.next()
                    P.op("act", lambda e, rs=rs, bv=bv: e.activation(out=rs[:], in_=bv[:], func=AF.Sqrt, bias=epsT[:], scale=1.0),
                         reads=[bvk, "epsT"], writes=[rsk])
                    P.op("dve", lambda e, rs=rs: e.reciprocal(out=rs[:], in_=rs[:]), reads=[rsk], writes=[rsk])
                    P.op("dve", lambda e, rs=rs, cen=cen: e.tensor_tensor(out=cen[:], in0=cen[:], in1=rs[:], op=ALU.mult),
                         reads=[rsk, cenk], writes=[cenk])
                    P.op("dve", lambda e, cen=cen: e.tensor_scalar(out=cen[:], in0=cen[:], scalar1=kng[:, 0:1], scalar2=knb[:, 0:1],
                                                                   op0=ALU.mult, op1=ALU.add),
                         reads=[cenk, "kng", "knb"], writes=[cenk])
                    bp, bpk = bank_gen()
                    P.op("pe", lambda e, bp=bp, cen=cen: e.matmul(bp[:], Rperm[:], cen[:], start=True, stop=True),
                         reads=["Rperm", cenk], writes=[bpk])
                    u1, u1k = t1r.next()
                    P.op("dve", lambda e, u1=u1, cen=cen, cs=cs: e.tensor_tensor(out=u1[:], in0=cen[:], in1=cs[:, 0, :], op=ALU.mult),
                         reads=[cenk, ck], writes=[u1k])
                    u2, u2k = t2r.next()
                    P.op("dve", lambda e, u2=u2, bp=bp, cs=cs: e.tensor_tensor(out=u2[:], in0=bp[:], in1=cs[:, 1, :], op=ALU.mult),
                         reads=[bpk, ck], writes=[u2k])
                    P.op("pool", lambda e, u1=u1, u2=u2, tt=tt: e.tensor_tensor(
                        out=KI[:, tt * 512:(tt + 1) * 512], in0=u1[:], in1=u2[:], op=ALU.add),
                        reads=[u1k, u2k], writes=[("KI", tt)])

                gen_mode[0] = 2
                load_w(wbig, "wbig", C_QA, 8)
                load_w(wmid, "wmid", C_QI, 4)
                P.dma("pool", lambda e, l=l: e.dma_start(out=wwi[:], in_=wt[l, :, 1024:1028].rearrange("(c p) n -> p c n", p=128)),
                      writes=["wwi"])
                w_scale = 4 ** -0.5 * 64 ** -0.5
                for tt in range(NT):
                    xt, xk, cs, ck = load_tile(tt)
                    for c in range(4):
                        rope_chunk(wbig, "wbig", c, 4 + c, xt, xk, cs, ck, QA[:, c, :], "QA")
                    for c in range(2):
                        rope_chunk(wmid, "wmid", c, 2 + c, xt, xk, cs, ck, QI[:, c, :], "QI")
                    bt, bk = bank_gen()

                    def fwi(e, bt=bt, xt=xt):
                        first = True
                        for j in range(4):
                            for dc in range(8):
                                ins = e.matmul(bt[:, j * 4:(j + 1) * 4], xt[:, dc, j * 128:(j + 1) * 128], wwi[:, dc, :],
                                               start=first, stop=(j == 3 and dc == 7))
                                first = False
                        return ins
                    P.op("pe", fwi, reads=[xk, "wwi"], writes=[bk])
                    P.op("dve", lambda e, bt=bt: e.tensor_copy(wiT[:].rearrange("p a b -> p (a b)"), bt[:, 0:16]), reads=[bk], writes=["wiT"])
                    P.op("act", lambda e: e.activation(out=absw[:], in_=wiT[:], func=AF.Abs, scale=w_scale),
                         reads=["wiT"], writes=["absw"])
                    P.op("dve", lambda e: e.tensor_scalar(out=sgnw[:], in0=wiT[:], scalar1=0.0, scalar2=2.0,
                                                          op0=ALU.is_ge, op1=ALU.mult), reads=["wiT"], writes=["sgnw"])
                    P.op("dve", lambda e: e.tensor_scalar(out=sgnw[:], in0=sgnw[:], scalar1=-1.0, scalar2=None,
                                                          op0=ALU.add), reads=["sgnw"], writes=["sgnw"])
                    for j in range(4):
                        i = tt * 4 + j
                        n = (i + 1) * 128
                        tc0 = j * 128
                        for kc in range(0, n, 512):
                            w_ = min(512, n - kc)
                            for h in range(4):
                                g = h % 2
                                bt, bk = bank_par(g)
                                P.op("pe", lambda e, bt=bt, g=g, h=h, kc=kc, w_=w_, tc0=tc0: e.matmul(
                                    bt[:, 0:w_], QI[g * 64:(g + 1) * 64, h // 2, tc0:tc0 + 128],
                                    KI[g * 64:(g + 1) * 64, kc:kc + w_], start=True, stop=True),
                                    reads=["QI"] + [("KI", t_) for t_ in range(kc // 512, (kc + w_ + 511) // 512)], writes=[bk])
                                r_, rk = rr.next()
                                P.op("act", lambda e, bt=bt, r_=r_, w_=w_, j=j, h=h: e.activation(
                                    out=r_[:, 0:w_], in_=bt[:, 0:w_], func=AF.Relu, scale=absw[:, j, h:h + 1]),
                                    reads=[bk, "absw"], writes=[rk])
                                if h == 0:
                                    P.op("dve", lambda e, r_=r_, w_=w_, kc=kc, j=j: e.tensor_scalar(
                                        out=IS[:, kc:kc + w_], in0=r_[:, 0:w_], scalar1=sgnw[:, j, 0:1], scalar2=None, op0=ALU.mult),
                                        reads=[rk, "sgnw"], writes=["IS"])
                                else:
                                    P.op("dve", lambda e, r_=r_, w_=w_, kc=kc, j=j, h=h: e.scalar_tensor_tensor(
                                        out=IS[:, kc:kc + w_], in0=r_[:, 0:w_], scalar=sgnw[:, j, h:h + 1], in1=IS[:, kc:kc + w_],
                                        op0=ALU.mult, op1=ALU.add), reads=[rk, "sgnw", "IS"], writes=["IS"])
                        P.op("pool", lambda e, i=i: e.affine_select(
                            out=IS[:, i * 128:(i + 1) * 128], in_=IS[:, i * 128:(i + 1) * 128], pattern=[[-1, 128]],
                            compare_op=ALU.is_ge, fill=P.fill(e, NEG), base=0, channel_multiplier=1), reads=["IS"], writes=["IS"])
                        if debug and l == 0:
                            P.dma("sp", lambda e, i=i, n=n: e.dma_start(out=is_d[i * 128:(i + 1) * 128, 0:n], in_=IS[:, 0:n]),
                                  reads=["IS"], writes=[("is_d", i)])
                        rounds = min(NSEL, n) // 8
                        for r in range(rounds):
                            P.op("dve", lambda e, n=n: e.max(out=m8[:], in_=IS[:, 0:n]), reads=["IS"], writes=["m8"])
                            P.op("dve", lambda e, n=n: e.match_replace(out=IS[:, 0:n], in_to_replace=m8[:], in_values=IS[:, 0:n],
                                                                       imm_value=NEG), reads=["IS", "m8"], writes=["IS"])
                        P.op("dve", lambda e, n=n: e.tensor_scalar(out=Mk[:, 0:n], in0=IS[:, 0:n], scalar1=NEG, scalar2=None,
                                                                   op0=ALU.is_equal), reads=["IS"], writes=["Mk"])
                        P.op("pool", lambda e, i=i: e.affine_select(
                            out=Mk[:, i * 128:(i + 1) * 128], in_=Mk[:, i * 128:(i + 1) * 128], pattern=[[-1, 128]],
                            compare_op=ALU.is_ge, fill=P.fill(e, 0.0), base=0, channel_multiplier=1), reads=["Mk"], writes=["Mk"])
                        if debug and l == 0:
                            P.dma("sp", lambda e, i=i, n=n: e.dma_start(out=mk_d[i * 128:(i + 1) * 128, 0:n], in_=Mk[:, 0:n]),
                                  reads=["Mk"], writes=[("mk_d", i)])
                        for k0_ in range(0, i + 1, 8):
                            nk = min(8, i + 1 - k0_)
                            bt, bk = bank_gen()
                            btb = bt[:].bitcast(BF16)

                            def ftr(e, btb=btb, k0_=k0_, nk=nk):
                                for q in range(nk):
                                    ins = e.transpose(btb[:, q * 128:(q + 1) * 128], Mk[:, (k0_ + q) * 128:(k0_ + q + 1) * 128], identB[:])
                                return ins
                            P.op("pe", ftr, reads=["Mk", "identB"], writes=[bk])
                            P.op("act", lambda e, btb=btb, k0_=k0_, nk=nk: e.activation(
                                out=MT[:, k0_:k0_ + nk, :], in_=btb[:, 0:nk * 128].rearrange("p (k t) -> p k t", k=nk), func=AF.Copy),
                                reads=[bk], writes=["MT"])
                        Y, Yk = Yr.next()
                        for g in range(2):
                            bo, bok = bank_o()
                            for kb in range(i + 1):
                                bs, bsk = bank_par(g)

                                def fqk(e, bs=bs, g=g, kb=kb, tc0=tc0):
                                    for hh in range(4):
                                        ins = e.matmul(bs[:, hh * 128:(hh + 1) * 128],
                                                       KA[g * 64:(g + 1) * 64, hh, kb * 128:(kb + 1) * 128],
                                                       QA[g * 64:(g + 1) * 64, hh, tc0:tc0 + 128], start=True, stop=True)
                                    return ins
                                P.op("pe", fqk, reads=[("KA", kb // 4), "QA"], writes=[bsk])
                                E, Ek = Er.next()
                                P.op("act", lambda e, E=E, bs=bs: e.activation(
                                    out=E[:].rearrange("p h t -> p (h t)"), in_=bs[:], func=AF.Exp, scale=0.125),
                                    reads=[bsk], writes=[Ek])
                                P.op("dve", lambda e, E=E, kb=kb: e.tensor_tensor(
                                    out=E[:], in0=E[:], in1=MT[:, kb, :].unsqueeze(1).to_broadcast([128, 4, 128]), op=ALU.mult),
                                    reads=[Ek, "MT"], writes=[Ek])

                                def fav(e, E=E, bo=bo, g=g, kb=kb, i=i):
                                    for hh in range(4):
                                        ins = e.matmul(bo[:, hh * 65:(hh + 1) * 65], E[:, hh, :], VA[:, kb, 2 * hh + g, :],
                                                       start=(kb == 0 and hh == 0), stop=(kb == i and hh == 3), skip_group_check=True)
                                    return ins
                                P.op("pe", fav, reads=[Ek, ("VA", kb), "VAones"], writes=[bok])
                            o3 = bo[:, 0:260].rearrange("p (h d) -> p h d", h=4)
                            P.op("dve", lambda e, o3=o3: e.reciprocal(out=rden[:], in_=o3[:, :, 64]), reads=[bok], writes=["rdenA"])
                            y3 = Y[:].rearrange("p (h g d) -> p h g d", h=4, g=2)
                            P.op("dve", lambda e, o3=o3, y3=y3, g=g: e.tensor_tensor(
                                out=y3[:, :, g, :], in0=o3[:, :, 0:64], in1=rden[:].unsqueeze(2).to_broadcast([128, 4, 64]), op=ALU.mult),
                                reads=[bok, "rdenA"], writes=[Yk])
                        bt, bk = bank_gen()
                        btb = bt[:].bitcast(BF16)

                        def fty(e, btb=btb, Y=Y):
                            for c in range(4):
                                ins = e.transpose(btb[:, c * 128:(c + 1) * 128], Y[:, c * 128:(c + 1) * 128], identB[:])
                            return ins
                        P.op("pe", fty, reads=[Yk, "identB"], writes=[bk])
                        YT, YTk = YTr.next()
                        P.op("act", lambda e, btb=btb, YT=YT: e.activation(
                            out=YT[:], in_=btb[:, 0:512].rearrange("p (c t) -> p c t", c=4), func=AF.Copy), reads=[bk], writes=[YTk])
                        P.dma("sp", lambda e, YT=YT, i=i: e.dma_start(out=yaT_d[:, :, i * 128:(i + 1) * 128], in_=YT[:]),
                              reads=[YTk], writes=[("yaT_d", i)])
                P.barrier_all()
                P.emit(sems)
                gen_mode[0] = 4
            if stop_after == "A":
                break

            with ExitStack() as ph:
                sb = lambda name, shape, dt: ph.enter_context(nc.sbuf_tensor(uniq(name), shape, dt))
                KB = sb("KB", [128, 4, S], BF16)
                VB = sb("VB", [128, NB, 512], BF16)
                wk_ = sb("wkB", [128, 8, 512], BF16)
                wv_ = sb("wvB", [128, 8, 512], BF16)
                xtr = Ring([sb("xtB%d" % i, [128, 8, 512], BF16) for i in range(2)], "xtB")
                QB = sb("QB", [128, 4, 512], BF16)
                er = Ring([sb("eB%d" % i, [128, 512], F32) for i in range(2)], "eB")
                spr = Ring([sb("spB%d" % i, [128, 512], F32) for i in range(3)], "spB")
                Lr = Ring([sb("LB%d" % i, [128, 512], BF16) for i in range(3)], "LB")
                Lsr = Ring([sb("LsB%d" % i, [128, 512], BF16) for i in range(2)], "LsB")
                Ar = Ring([sb("AB%d" % i, [128, 512], F32) for i in range(2)], "AB")
                atr = Ring([sb("atB%d" % i, [128, 4, 128], BF16) for i in range(3)], "atB")
                Yr = Ring([sb("YB%d" % i, [128, 512], BF16) for i in range(2)], "YB")
                YTr = Ring([sb("YTB%d" % i, [128, 4, 128], BF16) for i in range(2)], "YTB")

                P.dma("pool", lambda e, l=l: e.dma_start(
                    out=wk_[:], in_=wf[l, :, C_KB * 128:(C_KB + 4) * 128].rearrange("(c p) n -> p c n", p=128)), writes=["wkB"])
                P.dma("pool", lambda e, l=l: e.dma_start(out=wv_[:], in_=wt[l, :, 512:1024].rearrange("(c p) n -> p c n", p=128)),
                      writes=["wvB"])
                for tt in range(NT):
                    xt, xk = xtr.next()
                    P.dma("sp", lambda e, xt=xt, tt=tt: e.dma_start(out=xt[:], in_=xT_d[:, :, tt * 512:(tt + 1) * 512]),
                          reads=[("xT_d", tt)], writes=[xk])
                    for c in range(4):
                        bt, bk = bank_gen()
                        proj_fm(bt, bk, wk_, "wkB", c, xt, xk)
                        P.op("act", lambda e, bt=bt, c=c, tt=tt: e.activation(out=KB[:, c, tt * 512:(tt + 1) * 512], in_=bt[:], func=AF.Copy),
                             reads=[bk], writes=[("KB", tt)])
                    for j in range(4):
                        b = tt * 4 + j
                        bt, bk = bank_gen()

                        def fv(e, bt=bt, j=j, xt=xt):
                            for dc in range(8):
                                ins = e.matmul(bt[:], xt[:, dc, j * 128:(j + 1) * 128], wv_[:, dc, :], start=(dc == 0), stop=(dc == 7))
                            return ins
                        P.op("pe", fv, reads=[xk, "wvB"], writes=[bk])
                        P.op("dve", lambda e, bt=bt, b=b: e.tensor_copy(VB[:, b, :], bt[:]), reads=[bk], writes=[("VB", b)])
                gen_mode[0] = 2
                P.dma("pool", lambda e, l=l: e.dma_start(
                    out=wk_[:], in_=wf[l, :, C_QB * 128:(C_QB + 4) * 128].rearrange("(c p) n -> p c n", p=128)), writes=["wkB"])
                for tt in range(NT):
                    xt, xk = xtr.next()
                    P.dma("sp", lambda e, xt=xt, tt=tt: e.dma_start(out=xt[:], in_=xT_d[:, :, tt * 512:(tt + 1) * 512]),
                          reads=[("xT_d", tt)], writes=[xk])
                    for c in range(4):
                        bt, bk = bank_gen()
                        proj_fm(bt, bk, wk_, "wkB", c, xt, xk)
                        P.op("act", lambda e, bt=bt, c=c: e.activation(out=QB[:, c, :], in_=bt[:], func=AF.Copy), reads=[bk], writes=["QB"])
                    for j in range(4):
                        i = tt * 4 + j
                        tc0 = j * 128
                        Y, Yk = Yr.next()
                        for g in range(2):
                            bo, bok = bank_o()
                            Lsp, Lspk = None, None
                            for kb in range(i, -1, -1):
                                diag = (kb == i)
                                bs, bsk = bank_par(g)

                                def fqk(e, bs=bs, g=g, kb=kb, tc0=tc0):
                                    for hh in range(4):
                                        ins = e.matmul(bs[:, hh * 128:(hh + 1) * 128],
                                                       KB[g * 64:(g + 1) * 64, hh, kb * 128:(kb + 1) * 128],
                                                       QB[g * 64:(g + 1) * 64, hh, tc0:tc0 + 128], start=True, stop=True)
                                    return ins
                                P.op("pe", fqk, reads=[("KB", kb // 4), "QB"], writes=[bsk])
                                ee, eek = er.next()
                                P.op("act", lambda e, ee=ee, bs=bs: e.activation(out=ee[:], in_=bs[:], func=AF.Exp, scale=-0.125),
                                     reads=[bsk], writes=[eek])
                                sp_, spk = spr.next()
                                P.op("act", lambda e, ee=ee, sp_=sp_: e.activation(out=sp_[:], in_=ee[:], func=AF.Ln, bias=1.0, scale=1.0),
                                     reads=[eek], writes=[spk])
                                Lt, Lk = Lr.next()
                                P.op("dve", lambda e, Lt=Lt, bs=bs, sp_=sp_: e.scalar_tensor_tensor(
                                    out=Lt[:], in0=bs[:], scalar=-0.125, in1=sp_[:], op0=ALU.mult, op1=ALU.subtract),
                                    reads=[bsk, spk], writes=[Lk])
                                if diag:
                                    P.op("pool", lambda e, Lt=Lt: e.affine_select(
                                        out=Lt[:].rearrange("p (h t) -> p h t", h=4), in_=Lt[:].rearrange("p (h t) -> p h t", h=4),
                                        pattern=[[0, 4], [1, 128]], compare_op=ALU.is_ge, fill=P.fill(e, 0.0), base=-1, channel_multiplier=-1),
                                        reads=[Lk], writes=[Lk])
                                bc, bck = bank_gen()

                                def fcs(e, bc=bc, Lt=Lt, Lsp=Lsp):
                                    ins = e.matmul(bc[:], Ubf[:], Lt[:], start=True, stop=(Lsp is None))
                                    if Lsp is not None:
                                        ins = e.matmul(bc[:], onesB[:], Lsp[:], start=False, stop=True)
                                    return ins
                                P.op("pe", fcs, reads=["Ubf", "onesB", Lk] + ([Lspk] if Lsp is not None else []), writes=[bck])
                                if kb > 0:
                                    Ls, Lsk = Lsr.next()
                                    if Lsp is None:
                                        P.op("pool", lambda e, Ls=Ls, Lt=Lt: e.tensor_copy(Ls[:], Lt[:]), reads=[Lk], writes=[Lsk])
                                    else:
                                        P.op("pool", lambda e, Ls=Ls, Lt=Lt, Lsp=Lsp: e.tensor_tensor(out=Ls[:], in0=Lsp[:], in1=Lt[:], op=ALU.add),
                                             reads=[Lk, Lspk], writes=[Lsk])
                                    Lsp, Lspk = Ls, Lsk
                                A_, Ak = Ar.next()
                                P.op("dve", lambda e, A_=A_, bc=bc, sp_=sp_: e.tensor_tensor(out=A_[:], in0=bc[:], in1=sp_[:], op=ALU.subtract),
                                     reads=[bck, spk], writes=[Ak])
                                at, atk = atr.next()
                                P.op("act", lambda e, at=at, A_=A_: e.activation(out=at[:].rearrange("p h t -> p (h t)"), in_=A_[:], func=AF.Exp),
                                     reads=[Ak], writes=[atk])
                                if diag:
                                    P.op("pool", lambda e, at=at: e.affine_select(
                                        out=at[:], in_=at[:], pattern=[[0, 4], [1, 128]], compare_op=ALU.is_ge, fill=P.fill(e, 0.0),
                                        base=-1, channel_multiplier=-1), reads=[atk], writes=[atk])

                                def fav(e, at=at, bo=bo, g=g, kb=kb, i=i):
                                    for hh in range(4):
                                        h = 2 * hh + g
                                        ins = e.matmul(bo[:, hh * 64:(hh + 1) * 64], at[:, hh, :], VB[:, kb, h * 64:(h + 1) * 64],
                                                       start=(kb == i and hh == 0), stop=(kb == 0 and hh == 3), skip_group_check=True)
                                    return ins
                                P.op("pe", fav, reads=[atk, ("VB", kb)], writes=[bok])
                            y3 = Y[:].rearrange("p (h g d) -> p h g d", h=4, g=2)
                            P.op("act", lambda e, bo=bo, y3=y3, g=g: e.activation(
                                out=y3[:, :, g, :], in_=bo[:, 0:256].rearrange("p (h d) -> p h d", h=4), func=AF.Copy),
                                reads=[bok], writes=[Yk])
                        bt, bk = bank_gen()
                        btb = bt[:].bitcast(BF16)

                        def fty(e, btb=btb, Y=Y):
                            for c in range(4):
                                ins = e.transpose(btb[:, c * 128:(c + 1) * 128], Y[:, c * 128:(c + 1) * 128], identB[:])
                            return ins
                        P.op("pe", fty, reads=[Yk, "identB"], writes=[bk])
                        YT, YTk = YTr.next()
                        P.op("act", lambda e, btb=btb, YT=YT: e.activation(
                            out=YT[:], in_=btb[:, 0:512].rearrange("p (c t) -> p c t", c=4), func=AF.Copy), reads=[bk], writes=[YTk])
                        P.dma("sp", lambda e, YT=YT, i=i: e.dma_start(out=ybT_d[:, :, i * 128:(i + 1) * 128], in_=YT[:]),
                              reads=[YTk], writes=[("ybT_d", i)])
                P.barrier_all()
                P.emit(sems)
                gen_mode[0] = 4
            if stop_after == "B":
                break

            with ExitStack() as ph:
                sb = lambda name, shape, dt: ph.enter_context(nc.sbuf_tensor(uniq(name), shape, dt))
                wg_ = sb("wgC", [128, 8, 2048], BF16)
                wa_ = sb("waC", [128, 4, D], BF16)
                wb_ = sb("wbC", [128, 4, D], BF16)
                wo_ = sb("woC", [128, 8, D], BF16)
                bgT = sb("bgT", [128, 16], F32)
                gb = sb("gbC", [128, 4, D], F32)
                xtr = Ring([sb("xtC%d" % i, [128, 8, 512], BF16) for i in range(2)], "xtC")
                yar = Ring([sb("yaC%d" % i, [128, 4, 512], BF16) for i in range(2)], "yaC")
                ybr = Ring([sb("ybC%d" % i, [128, 4, 512], BF16) for i in range(2)], "ybC")
                G = sb("GC", [128, 16, 512], BF16)
                MG = sb("MGC", [128, 8, 512], BF16)
                m1r = Ring([sb("m1C%d" % i, [128, 512], F32) for i in range(2)], "m1C")
                m2r = Ring([sb("m2C%d" % i, [128, 512], F32) for i in range(2)], "m2C")
                xbr = Ring([sb("xbC%d" % i, [128, D], F32) for i in range(2)], "xbC")
                rr_ = Ring([sb("rC%d" % i, [128, D], F32) for i in range(2)], "rC")
                x1r = Ring([sb("x1C%d" % i, [128, D], F32) for i in range(2)], "x1C")
                xT32 = sb("xT32", [128, 8, 128], F32)
                xTt = Ring([sb("xTtC%d" % i, [128, 8, 512], BF16) for i in range(2)], "xTtC")
                st6 = sb("st6", [128, 2, 6], F32)
                mv = sb("mv", [128, 2], F32)
                rstd = sb("rstd", [128, 1], F32)
                small = (st6, mv, rstd)
                rt = {n_: sb("rt_" + n_, [128, NE], F32) for n_ in ("sc", "bi", "ing", "msk", "w")}
                r4 = {n_: sb("r4_" + n_, [128, 4], F32) for n_ in ("mab", "nab", "mcd", "ncd", "t1", "t2", "gs", "eq")}
                r1_ = {n_: sb("r1_" + n_, [128, 1], F32) for n_ in ("gm", "ws")}
                r8 = sb("r8", [128, 8], F32)

                for hf_ in range(2):
                    P.dma("pool", lambda e, l=l, hf_=hf_: e.dma_start(
                        out=wg_[:, :, hf_ * 1024:(hf_ + 1) * 1024],
                        in_=wf[l, :, (C_G + 8 * hf_) * 128:(C_G + 8 * hf_ + 8) * 128].rearrange("(c p) n -> p c n", p=128)), writes=["wgC"])
                P.dma("pool", lambda e, l=l: e.dma_start(out=wa_[:], in_=wa_d[l].rearrange("(c p) n -> p c n", p=128)), writes=["waC"])
                P.dma("pool", lambda e, l=l: e.dma_start(out=wb_[:], in_=wb_d[l].rearrange("(c p) n -> p c n", p=128)), writes=["wbC"])
                P.dma("pool", lambda e, l=l: e.dma_start(out=wo_[:], in_=wo_d[l].rearrange("(c p) n -> p c n", p=128)), writes=["woC"])
                P.dma("sp", lambda e, l=l: e.dma_start(out=bgT[:], in_=bg_d[l]), writes=["bgT"])
                for q in range(4):
                    P.dma("sp", lambda e, l=l, q=q: e.dma_start(out=gb[:, q, :], in_=ln_d[l, q].partition_broadcast(128)), writes=["gbC"])

                for tt in range(NT):
                    xt, xk = xtr.next()
                    P.dma("sp", lambda e, xt=xt, tt=tt: e.dma_start(out=xt[:], in_=xT_d[:, :, tt * 512:(tt + 1) * 512]),
                          reads=[("xT_d", tt)], writes=[xk])
                    ya, yak = yar.next()
                    P.dma("sp", lambda e, ya=ya, tt=tt: e.dma_start(out=ya[:], in_=yaT_d[:, :, tt * 512:(tt + 1) * 512]),
                          reads=[("yaT_d", tt * 4 + q) for q in range(4)], writes=[yak])
                    yb, ybk = ybr.next()
                    P.dma("sp", lambda e, yb=yb, tt=tt: e.dma_start(out=yb[:], in_=ybT_d[:, :, tt * 512:(tt + 1) * 512]),
                          reads=[("ybT_d", tt * 4 + q) for q in range(4)], writes=[ybk])
                    KCUT = int(os.environ.get("KCUT", "99"))
                    if KCUT < 2:
                        continue
                    for c in range(16):
                        bt, bk = bank_gen()
                        proj_fm(bt, bk, wg_, "wgC", c, xt, xk)
                        P.op("act", lambda e, bt=bt, c=c: e.activation(out=G[:, c, :], in_=bt[:], func=AF.Sigmoid, bias=bgT[:, c:c + 1], scale=1.0),
                             reads=[bk, "bgT"], writes=[("GC", c)])
                    if KCUT < 3:
                        continue
                    for c in range(8):
                        ba, bak = bank_gen()

                        def fa(e, ba=ba, c=c, ya=ya):
                            for ec in range(4):
                                ins = e.matmul(ba[:], wa_[:, ec, c * 128:(c + 1) * 128], ya[:, ec, :], start=(ec == 0), stop=(ec == 3))
                            return ins
                        P.op("pe", fa, reads=["waC", yak], writes=[bak])
                        bb_, bbk = bank_gen()

                        def fb(e, bb_=bb_, c=c, yb=yb):
                            for ec in range(4):
                                ins = e.matmul(bb_[:], wb_[:, ec, c * 128:(c + 1) * 128], yb[:, ec, :], start=(ec == 0), stop=(ec == 3))
                            return ins
                        P.op("pe", fb, reads=["wbC", ybk], writes=[bbk])
                        m1, m1k = m1r.next()
                        m2, m2k = m2r.next()
                        P.op("dve", lambda e, m1=m1, ba=ba, c=c: e.tensor_tensor(out=m1[:], in0=ba[:], in1=G[:, c, :], op=ALU.mult),
                             reads=[bak, ("GC", c)], writes=[m1k])
                        P.op("dve", lambda e, m2=m2, bb_=bb_, c=c: e.tensor_tensor(out=m2[:], in0=bb_[:], in1=G[:, 8 + c, :], op=ALU.mult),
                             reads=[bbk, ("GC", 8 + c)], writes=[m2k])
                        P.op("pool", lambda e, m1=m1, m2=m2, c=c: e.tensor_tensor(out=MG[:, c, :], in0=m1[:], in1=m2[:], op=ALU.add),
                             reads=[m1k, m2k], writes=["MGC"])
                    if KCUT < 4:
                        continue
                    xTn, xTnk = xTt.next()
                    for j in range(4):
                        b = tt * 4 + j
                        xb, xbk = xbr.next()
                        P.dma("sp", lambda e, xb=xb, b=b: e.dma_start(out=xb[:], in_=xsrc[b * 128:(b + 1) * 128, :]),
                              reads=[("xres_d", b)], writes=[xbk])
                        r_, rk = rr_.next()
                        for hf in range(2):
                            bo, bok = bank_gen()

                            def fo(e, bo=bo, j=j, hf=hf):
                                for c in range(8):
                                    ins = e.matmul(bo[:], MG[:, c, j * 128:(j + 1) * 128], wo_[:, c, hf * 512:(hf + 1) * 512],
                                                   start=(c == 0), stop=(c == 7))
                                return ins
                            P.op("pe", fo, reads=["MGC", "woC"], writes=[bok])
                            P.op("dve", lambda e, r_=r_, xb=xb, bo=bo, hf=hf: e.scalar_tensor_tensor(
                                out=r_[:, hf * 512:(hf + 1) * 512], in0=xb[:, hf * 512:(hf + 1) * 512], scalar=DN_ALPHA, in1=bo[:],
                                op0=ALU.mult, op1=ALU.add), reads=[xbk, bok], writes=[rk])
                        if KCUT < 5:
                            continue
                        x1, x1k = x1r.next()
                        layer_norm_block(ph, r_, rk, gb, "gbC", 0, x1, x1k, small)
                        P.dma("sp", lambda e, x1=x1, b=b: e.dma_start(out=xres_d[b * 128:(b + 1) * 128, :], in_=x1[:]),
                              reads=[x1k], writes=[("xres_d", b)])
                        if KCUT < 6:
                            continue
                        transposes_f32(x1, x1k, xTn[:, :, j * 128:(j + 1) * 128], xTnk, xT32, "xT32")
                        if KCUT < 7:
                            continue
                        bt, bk = bank_gen()

                        def frt(e, bt=bt):
                            for c in range(8):
                                ins = e.matmul(bt[:, 0:NE], xT32[:, c, :], wr_sb[:, c, :], start=(c == 0), stop=(c == 7))
                            return ins
                        P.op("pe", frt, reads=[("xT32", 0), ("xT32", 1), "wr_sb"], writes=[bk])
                        sc, bi, ing, msk, w_ = rt["sc"], rt["bi"], rt["ing"], rt["msk"], rt["w"]
                        P.op("act", lambda e, bt=bt: e.activation(out=sc[:], in_=bt[:, 0:NE], func=AF.Sigmoid), reads=[bk], writes=["rt"])
                        if KCUT < 8:
                            continue
                        V = lambda fn: P.op("dve", fn, reads=["rt"], writes=["rt"])
                        V(lambda e: e.tensor_tensor(out=bi[:], in0=sc[:], in1=rb_sb[:], op=ALU.add))
                        b3 = bi[:].rearrange("p (g k) -> p g k", g=4)
                        V(lambda e: e.tensor_tensor(out=r4["mab"][:], in0=b3[:, :, 0], in1=b3[:, :, 1], op=ALU.max))
                        V(lambda e: e.tensor_tensor(out=r4["nab"][:], in0=b3[:, :, 0], in1=b3[:, :, 1], op=ALU.min))
                        V(lambda e: e.tensor_tensor(out=r4["mcd"][:], in0=b3[:, :, 2], in1=b3[:, :, 3], op=ALU.max))
                        V(lambda e: e.tensor_tensor(out=r4["ncd"][:], in0=b3[:, :, 2], in1=b3[:, :, 3], op=ALU.min))
                        V(lambda e: e.tensor_tensor(out=r4["t1"][:], in0=r4["mab"][:], in1=r4["mcd"][:], op=ALU.max))
                        V(lambda e: e.tensor_tensor(out=r4["t2"][:], in0=r4["mab"][:], in1=r4["mcd"][:], op=ALU.min))
                        V(lambda e: e.tensor_tensor(out=r4["eq"][:], in0=r4["nab"][:], in1=r4["ncd"][:], op=ALU.max))
                        V(lambda e: e.tensor_tensor(out=r4["t2"][:], in0=r4["t2"][:], in1=r4["eq"][:], op=ALU.max))
                        V(lambda e: e.tensor_tensor(out=r4["gs"][:], in0=r4["t1"][:], in1=r4["t2"][:], op=ALU.add))
                        V(lambda e: e.tensor_reduce(out=r1_["gm"][:], in_=r4["gs"][:], axis=AX.X, op=ALU.max))
                        V(lambda e: e.tensor_scalar(out=r4["eq"][:], in0=r4["gs"][:], scalar1=r1_["gm"][:, 0:1], scalar2=None, op0=ALU.is_ge))
                        i3 = ing[:].rearrange("p (g k) -> p g k", g=4)
                        V(lambda e: e.tensor_copy(i3, r4["eq"][:].unsqueeze(2).to_broadcast([128, 4, 4])))
                        V(lambda e: e.tensor_tensor(out=msk[:], in0=bi[:], in1=ing[:], op=ALU.mult))
                        V(lambda e: e.tensor_scalar(out=w_[:], in0=ing[:], scalar1=BIG, scalar2=-BIG, op0=ALU.mult, op1=ALU.add))
                        V(lambda e: e.tensor_tensor(out=msk[:], in0=msk[:], in1=w_[:], op=ALU.add))
                        V(lambda e: e.max(out=r8[:], in_=msk[:]))
                        V(lambda e: e.tensor_scalar(out=msk[:], in0=msk[:], scalar1=r8[:, 1:2], scalar2=None, op0=ALU.is_ge))
                        V(lambda e: e.tensor_tensor(out=w_[:], in0=sc[:], in1=msk[:], op=ALU.mult))
                        V(lambda e: e.tensor_reduce(out=r1_["ws"][:], in_=w_[:], axis=AX.X, op=ALU.add))
                        V(lambda e: e.reciprocal(out=r1_["ws"][:], in_=r1_["ws"][:]))
                        P.op("dve", lambda e, b=b: e.tensor_scalar(out=comb[:, b, :], in0=w_[:], scalar1=r1_["ws"][:, 0:1], scalar2=None, op0=ALU.mult),
                             reads=["rt"], writes=[("comb", b)])
                    if KCUT < 6:
                        continue
                    P.dma("sp", lambda e, xTn=xTn, tt=tt: e.dma_start(out=xT_d[:, :, tt * 512:(tt + 1) * 512], in_=xTn[:]),
                          reads=[xTnk], writes=[("xT_d", tt)])
                P.barrier_all()
                P.emit(sems)
            if stop_after == "C":
                break

            with ExitStack() as ph:
                sb = lambda name, shape, dt: ph.enter_context(nc.sbuf_tensor(uniq(name), shape, dt))
                NSUP = max(1, S // 2048)
                TS = S // NSUP
                NTB = TS // 128
                NTT = TS // 512
                XTs = sb("XTs", [128, 8, TS], BF16)
                yacc = sb("yacc", [128, NTB, D], F32)
                wgr = Ring([sb("wgD%d" % i, [128, 8, FE], BF16) for i in range(2)], "wgD")
                wur = Ring([sb("wuD%d" % i, [128, 8, FE], BF16) for i in range(2)], "wuD")
                wdr = Ring([sb("wdD%d" % i, [128, 4, D], BF16) for i in range(2)], "wdD")
                Hr = Ring([sb("HD%d" % i, [128, 4, 512], BF16) for i in range(2)], "HD")
                sgr = Ring([sb("sgD%d" % i, [128, 512], F32) for i in range(2)], "sgD")
                gb = sb("gbD", [128, 2, D], F32)
                xbr = Ring([sb("xbD%d" % i, [128, D], F32) for i in range(2)], "xbD")
                x2r = Ring([sb("x2D%d" % i, [128, D], F32) for i in range(2)], "x2D")
                xTt = Ring([sb("xTtD%d" % i, [128, 8, 512], BF16) for i in range(2)], "xTtD")
                st6 = sb("st6", [128, 2, 6], F32)
                mv = sb("mv", [128, 2], F32)
                rstd = sb("rstd", [128, 1], F32)
                small = (st6, mv, rstd)
                for q in range(2):
                    P.dma("sp", lambda e, l=l, q=q: e.dma_start(out=gb[:, q, :], in_=ln_d[l, 2 + q].partition_broadcast(128)), writes=["gbD"])
                for su in range(NSUP):
                    for tq in range(NTT):
                        tt = su * NTT + tq
                        P.dma("sp", lambda e, tq=tq, tt=tt: e.dma_start(out=XTs[:, :, tq * 512:(tq + 1) * 512], in_=xT_d[:, :, tt * 512:(tt + 1) * 512]),
                              reads=[("xT_d", tt)], writes=[("XTs", tq)])
                    for ex in range(NE):
                        wg_, wgk = wgr.next()
                        wu_, wuk = wur.next()
                        wd_, wdk = wdr.next()
                        P.dma("pool", lambda e, wg_=wg_, l=l, ex=ex: e.dma_start(out=wg_[:], in_=eg_d[l, ex].rearrange("(c p) n -> p c n", p=128)), writes=[wgk])
                        P.dma("pool", lambda e, wu_=wu_, l=l, ex=ex: e.dma_start(out=wu_[:], in_=eu_d[l, ex].rearrange("(c p) n -> p c n", p=128)), writes=[wuk])
                        P.dma("pool", lambda e, wd_=wd_, l=l, ex=ex: e.dma_start(out=wd_[:], in_=ed_d[l, ex].rearrange("(c p) n -> p c n", p=128)), writes=[wdk])
                        for tq in range(NTT):
                            Ht, Hk = Hr.next()
                            for fc in range(4):
                                bgk_t, bgk = bank_gen()

                                def fg(e, bt=bgk_t, fc=fc, tq=tq, w=wg_):
                                    for dc in range(8):
                                        ins = e.matmul(bt[:], w[:, dc, fc * 128:(fc + 1) * 128], XTs[:, dc, tq * 512:(tq + 1) * 512],
                                                       start=(dc == 0), stop=(dc == 7))
                                    return ins
                                P.op("pe", fg, reads=[wgk, ("XTs", tq)], writes=[bgk])
                                bu_t, buk = bank_gen()

                                def fu(e, bt=bu_t, fc=fc, tq=tq, w=wu_):
                                    for dc in range(8):
                                        ins = e.matmul(bt[:], w[:, dc, fc * 128:(fc + 1) * 128], XTs[:, dc, tq * 512:(tq + 1) * 512],
                                                       start=(dc == 0), stop=(dc == 7))
                                    return ins
                                P.op("pe", fu, reads=[wuk, ("XTs", tq)], writes=[buk])
                                sg, sgk = sgr.next()
                                P.op("act", lambda e, sg=sg, bt=bgk_t: e.activation(out=sg[:], in_=bt[:], func=AF.Silu), reads=[bgk], writes=[sgk])
                                P.op("dve", lambda e, sg=sg, bt=bu_t, Ht=Ht, fc=fc: e.tensor_tensor(out=Ht[:, fc, :], in0=bt[:], in1=sg[:], op=ALU.mult),
                                     reads=[buk, sgk], writes=[Hk])
                            for j in range(4):
                                bl = tq * 4 + j
                                b = su * NTB + bl
                                for hf in range(2):
                                    by, byk = bank_gen()

                                    def fd(e, by=by, Ht=Ht, j=j, hf=hf, w=wd_):
                                        for fc in range(4):
                                            ins = e.matmul(by[:], Ht[:, fc, j * 128:(j + 1) * 128], w[:, fc, hf * 512:(hf + 1) * 512],
                                                           start=(fc == 0), stop=(fc == 3))
                                        return ins
                                    P.op("pe", fd, reads=[Hk, wdk], writes=[byk])
                                    if ex == 0:
                                        P.op("dve", lambda e, by=by, bl=bl, b=b, hf=hf, ex=ex: e.tensor_scalar(
                                            out=yacc[:, bl, hf * 512:(hf + 1) * 512], in0=by[:], scalar1=comb[:, b, ex:ex + 1], scalar2=None, op0=ALU.mult),
                                            reads=[byk, ("comb", b)], writes=[("yacc", bl, hf)])
                                    else:
                                        P.op("dve", lambda e, by=by, bl=bl, b=b, hf=hf, ex=ex: e.scalar_tensor_tensor(
                                            out=yacc[:, bl, hf * 512:(hf + 1) * 512], in0=by[:], scalar=comb[:, b, ex:ex + 1],
                                            in1=yacc[:, bl, hf * 512:(hf + 1) * 512], op0=ALU.mult, op1=ALU.add),
                                            reads=[byk, ("comb", b), ("yacc", bl, hf)], writes=[("yacc", bl, hf)])
                    for tq in range(NTT):
                        tt = su * NTT + tq
                        xTn, xTnk = xTt.next()
                        for j in range(4):
                            bl = tq * 4 + j
                            b = su * NTB + bl
                            xb, xbk = xbr.next()
                            P.dma("sp", lambda e, xb=xb, b=b: e.dma_start(out=xb[:], in_=xres_d[b * 128:(b + 1) * 128, :]),
                                  reads=[("xres_d", b)], writes=[xbk])
                            P.op("dve", lambda e, xb=xb, bl=bl: e.scalar_tensor_tensor(
                                out=xb[:], in0=xb[:], scalar=DN_ALPHA, in1=yacc[:, bl, :], op0=ALU.mult, op1=ALU.add),
                                reads=[xbk, ("yacc", bl, 0), ("yacc", bl, 1)], writes=[xbk])
                            x2, x2k = x2r.next()
                            layer_norm_block(ph, xb, xbk, gb, "gbD", 0, x2, x2k, small)
                            if last:
                                P.dma("sp", lambda e, x2=x2, b=b: e.dma_start(out=out_d[b * 128:(b + 1) * 128, :], in_=x2[:]),
                                      reads=[x2k], writes=[("out_d", b)])
                            else:
                                P.dma("sp", lambda e, x2=x2, b=b: e.dma_start(out=xres_d[b * 128:(b + 1) * 128, :], in_=x2[:]),
                                      reads=[x2k], writes=[("xres_d", b)])
                                transposes_f32(x2, x2k, xTn[:, :, j * 128:(j + 1) * 128], xTnk)
                        if not last:
                            P.dma("sp", lambda e, xTn=xTn, tt=tt: e.dma_start(out=xT_d[:, :, tt * 512:(tt + 1) * 512], in_=xTn[:]),
                                  reads=[xTnk], writes=[("xT_d", tt)])
                P.barrier_all()
                P.emit(sems)
        build_program.n_ops = P.n_ops
    dbg = {"xT_d": xT_d, "xres_d": xres_d, "yaT_d": yaT_d, "ybT_d": ybT_d}
    return nc, dbg


IN_SPLITS = (512, 512, 512, 256, 64, 4, 512, 512, 512, 2048)


def _perm64(n_heads):
    idx = []
    for h in range(n_heads):
        for d in range(64):
            idx.append(h * 64 + (d + 32) % 64)
    return np.array(idx)


def host_constants(S):
    ident = np.eye(128, dtype=np.float32)
    j = np.arange(128)[:, None]
    s = np.arange(128)[None, :]
    U = (j > s).astype(np.float32)
    blk = (j // 64 == s // 64).astype(np.float32) / 64.0
    perm = (np.arange(128) // 64) * 64 + (np.arange(128) % 64 + 32) % 64
    R = np.zeros((128, 128), np.float32)
    R[perm, np.arange(128)] = 1.0
    c_mats = np.stack([ident, U, blk, R]).astype(np.float32)
    half = 32
    inv_freq = (np.float32(10000.0) ** (-np.arange(half, dtype=np.float32) / np.float32(half))).astype(np.float32)
    ang = (np.arange(S, dtype=np.float32)[:, None] * inv_freq[None, :]).astype(np.float32)
    cos = np.cos(ang.astype(np.float64)).astype(np.float32)
    sin = np.sin(ang.astype(np.float64)).astype(np.float32)
    d = np.arange(128) % 64
    c_cos = cos[:, d % 32].T.copy()
    sgn = np.where(d < 32, -1.0, 1.0).astype(np.float32)[:, None]
    c_sin = (sin[:, d % 32].T * sgn).astype(np.float32).copy()
    return c_mats, np.ascontiguousarray(c_cos), np.ascontiguousarray(c_sin)


def host_weights(inp, L):
    pts = np.cumsum((0,) + IN_SPLITS)
    w_in = inp["w_in"]
    sl = lambda k: w_in[:, :, pts[k]:pts[k + 1]]
    qa, ka, va, qi, ki, wi, qb, kb, vb, gates = [sl(k) for k in range(10)]
    p8, p4 = _perm64(8), _perm64(4)
    wf = np.concatenate([qa, qa[:, :, p8], ka, ka[:, :, p8], qi, qi[:, :, p4], ki, ki, qb, kb, gates], axis=2)
    assert wf.shape[2] == NFC * 128
    wt = np.concatenate([va, vb, wi], axis=2)
    m = {
        "wf": np.ascontiguousarray(wf[:L], dtype=np.float32),
        "wt": np.ascontiguousarray(wt[:L], dtype=np.float32),
        "wa": np.ascontiguousarray(inp["w_branch_a"][:L]),
        "wb": np.ascontiguousarray(inp["w_branch_b"][:L]),
        "wo": np.ascontiguousarray(inp["w_out"][:L]),
        "bg": np.ascontiguousarray(inp["b_gate"][:L].reshape(L, 16, 128).transpose(0, 2, 1)),
        "kng": np.ascontiguousarray(np.concatenate([inp["idx_k_norm_g"], inp["idx_k_norm_g"]], axis=1)[:L]),
        "knb": np.ascontiguousarray(np.concatenate([inp["idx_k_norm_b"], inp["idx_k_norm_b"]], axis=1)[:L]),
        "lnp": np.ascontiguousarray(np.stack([inp["ln1_g"], inp["ln1_b"], inp["ln2_g"], inp["ln2_b"]], axis=1)[:L]),
        "wr": np.ascontiguousarray(inp["w_router"]),
        "rb": np.ascontiguousarray(inp["router_bias"]),
        "eg": np.ascontiguousarray(inp["exp_w_gate"][:L]),
        "eu": np.ascontiguousarray(inp["exp_w_up"][:L]),
        "ed": np.ascontiguousarray(inp["exp_w_down"][:L]),
    }
    return m


def kernel(**inputs):
    inp = {k: np.asarray(v) for k, v in inputs.items()}
    x = inp["x"]
    B, S, _ = x.shape
    L = inp["w_in"].shape[0]
    nc, _ = build_program(S, L=L, NSEL=min(256, S // 4))
    c_mats, c_cos, c_sin = host_constants(S)
    wm = host_weights(inp, L)
    in_maps = []
    for b in range(B):
        m = dict(wm)
        m["x"] = np.ascontiguousarray(x[b])
        m["c_mats"], m["c_cos"], m["c_sin"] = c_mats, c_cos, c_sin
        in_maps.append(m)
    res = run_bass_kernel_spmd(nc, in_maps, core_ids=list(range(B)))
    out = np.stack([np.asarray(r["out"]) for r in res.results], axis=0)
    return out.astype(np.float32)
```
